# Optimizing a Trainium2 kernel written in Bass

```python
import math
import jax, jax.numpy as jnp
from jax import lax
import numpy as np

D_MODEL = 1024
BATCH = 2
SEQ = 16384
DEPTH = 2

N_MIXERS = 2
N_MLA_LAYERS = (DEPTH + 1) // N_MIXERS
N_HYENA_LAYERS = DEPTH // N_MIXERS

MLA_HEADS = 8
QK_NOPE_DIM = 128
QK_ROPE_DIM = 64
V_HEAD_DIM = 128
Q_LORA_RANK = 384
KV_LORA_RANK = 256
MLA_IN_DIM = Q_LORA_RANK + KV_LORA_RANK + QK_ROPE_DIM
ROPE_THETA = 10000.0
Q_BLOCK = 128

HYENA_ORDER = 2
HYENA_EMB_DIM = 33
HYENA_FILTER_WIDTH = 64
HYENA_FAST_DECAY = 0.3
HYENA_SLOW_DECAY = 1.5
HYENA_TARGET = 1e-2
SHORT_CONV_WIDTH = 3

D_FF = 2816
FFN_CONV_WIDTH = 3

NORM_EPS = 1e-5
RMS_EPS = 1e-6
DEEPNORM_ALPHA = (2.0 * DEPTH) ** 0.25
DEEPNORM_BETA = (8.0 * DEPTH) ** -0.25

kernel_name = "hybrid_mla_hyena_convffn_deepnorm"


def layer_norm(x, g, b):
    xf = x.astype(jnp.float32)
    mu = jnp.mean(xf, -1, keepdims=True)
    var = jnp.mean(jnp.square(xf - mu), -1, keepdims=True)
    return ((xf - mu) * lax.rsqrt(var + NORM_EPS) * g.astype(jnp.float32) + b.astype(jnp.float32)).astype(x.dtype)


def rms_norm(x, g):
    xf = x.astype(jnp.float32)
    ms = jnp.mean(jnp.square(xf), -1, keepdims=True)
    return (xf * lax.rsqrt(ms + RMS_EPS) * g.astype(jnp.float32)).astype(x.dtype)


def dwconv_centred(x, w):
    k_width = w.shape[0]
    pad = k_width // 2
    s = x.shape[1]
    xp = jnp.pad(x, ((0, 0), (pad, pad), (0, 0)))
    return sum(xp[:, k:k + s] * w[k] for k in range(k_width))


def rope_tables(positions):
    inv = 1.0 / (ROPE_THETA ** (jnp.arange(0, QK_ROPE_DIM, 2, dtype=jnp.float32) / QK_ROPE_DIM))
    ang = positions.astype(jnp.float32)[..., None] * inv
    return jnp.cos(ang), jnp.sin(ang)


def apply_rope(x, cos, sin):
    x1, x2 = jnp.split(x.astype(jnp.float32), 2, axis=-1)
    return jnp.concatenate([x1 * cos - x2 * sin, x1 * sin + x2 * cos], -1).astype(x.dtype)


def mla_mixer(x, cos, sin, w_in, g_q, w_uq, g_kv, w_ukv, w_o):
    b, s, _ = x.shape
    h = x @ w_in
    c_q, c_kv, k_rope = jnp.split(h, [Q_LORA_RANK, Q_LORA_RANK + KV_LORA_RANK], axis=-1)
    q = (rms_norm(c_q, g_q) @ w_uq).reshape(b, s, MLA_HEADS, QK_NOPE_DIM + QK_ROPE_DIM)
    q_nope = q[..., :QK_NOPE_DIM]
    q_rope = apply_rope(q[..., QK_NOPE_DIM:], cos[:, :, None], sin[:, :, None])
    k_rope = apply_rope(k_rope, cos, sin)
    kv = (rms_norm(c_kv, g_kv) @ w_ukv).reshape(b, s, MLA_HEADS, QK_NOPE_DIM + V_HEAD_DIM)
    k_nope, v = kv[..., :QK_NOPE_DIM], kv[..., QK_NOPE_DIM:]
    scale = (QK_NOPE_DIM + QK_ROPE_DIM) ** -0.5
    nb = s // Q_BLOCK
    qn_blocks = q_nope.reshape(b, nb, Q_BLOCK, MLA_HEADS, QK_NOPE_DIM).transpose(1, 0, 2, 3, 4)
    qr_blocks = q_rope.reshape(b, nb, Q_BLOCK, MLA_HEADS, QK_ROPE_DIM).transpose(1, 0, 2, 3, 4)

    def attend(blk):
        qn, qr = blk
        sc = (jnp.einsum('bqhd,bkhd->bhqk', qn, k_nope)
              + jnp.einsum('bqhr,bkr->bhqk', qr, k_rope))
        p = jax.nn.softmax(sc.astype(jnp.float32) * scale, axis=-1).astype(v.dtype)
        return jnp.einsum('bhqk,bkhd->bqhd', p, v)

    o = lax.map(attend, (qn_blocks, qr_blocks))
    o = o.transpose(1, 0, 2, 3, 4).reshape(b, s, MLA_HEADS * V_HEAD_DIM)
    return o @ w_o


def hyena_positional_features(length):
    t = jnp.linspace(0.0, 1.0, length, dtype=jnp.float32)[:, None]
    bands = (HYENA_EMB_DIM - 1) // 2
    w = 2.0 * math.pi * jnp.arange(length, dtype=jnp.float32)[:, None] / length
    f = jnp.linspace(1e-4, bands - 1, bands, dtype=jnp.float32)[None, :]
    z = jnp.concatenate([t, jnp.cos(f * w), -jnp.sin(f * w)], axis=-1)
    return t, z


def hyena_filters(length, fw1, fb1, fw2, fb2, fw3, fb3, freq, fw_out):
    t, z = hyena_positional_features(length)
    freq = freq.astype(jnp.float32)
    h = jnp.sin(freq * (z @ fw1.astype(jnp.float32) + fb1.astype(jnp.float32)))
    h = jnp.sin(freq * (h @ fw2.astype(jnp.float32) + fb2.astype(jnp.float32)))
    h = jnp.sin(freq * (h @ fw3.astype(jnp.float32) + fb3.astype(jnp.float32)))
    h = h @ fw_out.astype(jnp.float32)
    deltas = jnp.abs(jnp.linspace(math.log(HYENA_FAST_DECAY) / HYENA_TARGET,
                                  math.log(HYENA_SLOW_DECAY) / HYENA_TARGET,
                                  D_MODEL, dtype=jnp.float32))
    decay = jnp.exp(-t * deltas)
    return h.reshape(length, HYENA_ORDER, 2, D_MODEL) * decay[:, None, None, :]


def two_sided_kernel(h_fwd, h_bwd):
    zero = jnp.zeros_like(h_fwd[:1])
    return jnp.concatenate([h_fwd.at[0].add(h_bwd[0]), zero, h_bwd[:0:-1]], axis=0)


def hyena_mixer(x, w_in, w_short, fw1, fb1, fw2, fb2, fw3, fb3, freq, fw_out, d_bias, w_o):
    b, length, _ = x.shape
    u = dwconv_centred(x @ w_in, w_short)
    v, g1, g2 = jnp.split(u, 3, axis=-1)
    filt = hyena_filters(length, fw1, fb1, fw2, fb2, fw3, fb3, freq, fw_out)
    n = 2 * length
    z = v.astype(jnp.float32)
    for o, gate in enumerate((g1, g2)):
        k_f = jnp.fft.rfft(two_sided_kernel(filt[:, o, 0], filt[:, o, 1]), n=n, axis=0)
        z_f = jnp.fft.rfft(z, n=n, axis=1)
        y = jnp.fft.irfft(z_f * k_f, n=n, axis=1)[:, :length]
        z = gate.astype(jnp.float32) * (y + d_bias[o].astype(jnp.float32) * z)
    return z.astype(x.dtype) @ w_o


def conv_ffn(x, w_up, w_conv, w_down):
    h = dwconv_centred(x @ w_up, w_conv)
    a, g = jnp.split(h, 2, axis=-1)
    return (jax.nn.silu(g) * a) @ w_down


def setup_inputs(seed: int = 0) -> dict:
    key = jax.random.key(seed)
    ks = iter(jax.random.split(key, 40))

    def nrm(shape, scale):
        return jax.random.normal(next(ks), shape, jnp.float32) * scale

    def gain(shape):
        return 1.0 + nrm(shape, 0.01)

    nm, nh = N_MLA_LAYERS, N_HYENA_LAYERS
    hf = HYENA_FILTER_WIDTH
    return {
        "x": nrm((BATCH, SEQ, D_MODEL), 1.0),
        "positions": jnp.broadcast_to(jnp.arange(SEQ, dtype=jnp.int32), (BATCH, SEQ)),
        "mla_w_in": nrm((nm, D_MODEL, MLA_IN_DIM), D_MODEL ** -0.5),
        "mla_g_q": gain((nm, Q_LORA_RANK)),
        "mla_w_uq": nrm((nm, Q_LORA_RANK, MLA_HEADS * (QK_NOPE_DIM + QK_ROPE_DIM)), Q_LORA_RANK ** -0.5),
        "mla_g_kv": gain((nm, KV_LORA_RANK)),
        "mla_w_ukv": nrm((nm, KV_LORA_RANK, MLA_HEADS * (QK_NOPE_DIM + V_HEAD_DIM)), KV_LORA_RANK ** -0.5),
        "mla_w_o": nrm((nm, MLA_HEADS * V_HEAD_DIM, D_MODEL), (MLA_HEADS * V_HEAD_DIM) ** -0.5 * DEEPNORM_BETA),
        "hy_w_in": nrm((nh, D_MODEL, 3 * D_MODEL), D_MODEL ** -0.5),
        "hy_w_short": nrm((nh, SHORT_CONV_WIDTH, 3 * D_MODEL), SHORT_CONV_WIDTH ** -0.5),
        "hy_fw1": nrm((nh, HYENA_EMB_DIM, hf), HYENA_EMB_DIM ** -0.5),
        "hy_fb1": nrm((nh, hf), 0.02),
        "hy_fw2": nrm((nh, hf, hf), hf ** -0.5),
        "hy_fb2": nrm((nh, hf), 0.02),
        "hy_fw3": nrm((nh, hf, hf), hf ** -0.5),
        "hy_fb3": nrm((nh, hf), 0.02),
        "hy_freq": gain((nh, hf)),
        "hy_fw_out": nrm((nh, hf, HYENA_ORDER * 2 * D_MODEL), hf ** -0.5),
        "hy_d_bias": nrm((nh, HYENA_ORDER, D_MODEL), 0.1),
        "hy_w_o": nrm((nh, D_MODEL, D_MODEL), D_MODEL ** -0.5 * DEEPNORM_BETA),
        "ffn_w_up": nrm((DEPTH, D_MODEL, 2 * D_FF), D_MODEL ** -0.5),
        "ffn_w_conv": nrm((DEPTH, FFN_CONV_WIDTH, 2 * D_FF), FFN_CONV_WIDTH ** -0.5),
        "ffn_w_down": nrm((DEPTH, D_FF, D_MODEL), D_FF ** -0.5 * DEEPNORM_BETA),
        "ln1_g": gain((DEPTH, D_MODEL)),
        "ln1_b": nrm((DEPTH, D_MODEL), 0.01),
        "ln2_g": gain((DEPTH, D_MODEL)),
        "ln2_b": nrm((DEPTH, D_MODEL), 0.01),
    }


def reference(x, positions, mla_w_in, mla_g_q, mla_w_uq, mla_g_kv, mla_w_ukv, mla_w_o,
              hy_w_in, hy_w_short, hy_fw1, hy_fb1, hy_fw2, hy_fb2, hy_fw3, hy_fb3, hy_freq,
              hy_fw_out, hy_d_bias, hy_w_o, ffn_w_up, ffn_w_conv, ffn_w_down,
              ln1_g, ln1_b, ln2_g, ln2_b):
    cos, sin = rope_tables(positions)
    for i in range(DEPTH):
        j = i // N_MIXERS
        if i % N_MIXERS == 0:
            m = mla_mixer(x, cos, sin, mla_w_in[j], mla_g_q[j], mla_w_uq[j],
                          mla_g_kv[j], mla_w_ukv[j], mla_w_o[j])
        else:
            m = hyena_mixer(x, hy_w_in[j], hy_w_short[j], hy_fw1[j], hy_fb1[j], hy_fw2[j],
                            hy_fb2[j], hy_fw3[j], hy_fb3[j], hy_freq[j], hy_fw_out[j],
                            hy_d_bias[j], hy_w_o[j])
        x = layer_norm(DEEPNORM_ALPHA * x + m, ln1_g[i], ln1_b[i])
        x = layer_norm(DEEPNORM_ALPHA * x + conv_ffn(x, ffn_w_up[i], ffn_w_conv[i], ffn_w_down[i]),
                       ln2_g[i], ln2_b[i])
    return x
```

```python
import contextlib
import numpy as np
import concourse.bass as bass
import concourse.mybir as mybir
from concourse.bass_utils import run_bass_kernel_spmd

F32 = mybir.dt.float32
BF16 = mybir.dt.bfloat16
I32 = mybir.dt.int32
ALU = mybir.AluOpType
AF = mybir.ActivationFunctionType
AX = mybir.AxisListType


class Reg:
    __slots__ = ("w", "r", "name")

    def __init__(self, name=""):
        self.w = None
        self.r = []
        self.name = name


class Sched:
    EPOCH = 16000
    NDMA = 6

    def __init__(self, nc, es, tag=""):
        self.nc = nc
        self.es = es
        self.tag = tag
        self.engs = ("pe", "act", "dve", "pool", "sp")
        self.streams = {e: [] for e in self.engs}
        self.count = {e: 0 for e in self.engs}
        self.sems = {}
        self.dsems = {}
        self.dcount = {}
        self.dissued = {e: 0 for e in self.engs}
        self.known = {e: {} for e in self.engs}
        self.final = []

    def _sem(self, key):
        if key not in self.sems:
            self.sems[key] = self.es.enter_context(
                self.nc.semaphore("s%s_%s_%d" % (self.tag, key[0], key[1])))
        return self.sems[key]

    def _deps(self, eng, reads, writes):
        toks = []
        pe = eng == "pe"
        for r in reads:
            if r.w is not None and not (pe and r.w[2] == "pe"):
                toks.append(r.w)
        for w in writes:
            if w.w is not None and not (pe and w.w[2] == "pe"):
                toks.append(w.w)
            for t in w.r:
                if t[2] != eng or t[3]:
                    toks.append(t)
        need = {}
        for t in toks:
            k = t[0]
            if need.get(k, 0) < t[1]:
                need[k] = t[1]
        out = []
        kn = self.known[eng]
        for k, v in need.items():
            if kn.get(k, 0) < v:
                kn[k] = v
                out.append((k, v))
        return out

    def op(self, eng, fn, reads=(), writes=()):
        waits = self._deps(eng, reads, writes)
        idx = self.count[eng]
        self.count[eng] += 1
        key = (eng, idx // self.EPOCH)
        self._sem(key)
        tok = (key, idx % self.EPOCH + 1, eng, False)
        for r in reads:
            r.r.append(tok)
        for w in writes:
            w.w = tok
            w.r = []
        self.streams[eng].append((waits, fn, key, 1))
        return tok

    def dma(self, q, out, in_, reads=(), writes=(), final=False, **kw):
        n = self.dissued[q]
        self.dissued[q] += 1
        key = ("d" + q, n % self.NDMA)
        self._sem(key)
        waits = self._deps(q, reads, writes)
        prev = self.dcount.get(key, 0)
        kn = self.known[q]
        if prev and kn.get(key, 0) < prev:
            kn[key] = prev
            waits.append((key, prev))
        val = prev + 16
        self.dcount[key] = val
        tok = (key, val, q, True)
        for r in reads:
            r.r.append(tok)
        for w in writes:
            w.w = tok
            w.r = []
        self.streams[q].append((waits, lambda e: e.dma_start(out=out, in_=in_, **kw), key, 16))
        if final:
            self.final.append(tok)
        return tok

    def emit(self):
        nc = self.nc
        handles = {"pe": "tensor", "act": "scalar", "dve": "vector", "pool": "gpsimd", "sp": "sync"}
        fin = {}
        for t in self.final:
            fin[t[0]] = max(fin.get(t[0], 0), t[1])
        with nc.Block() as block:
            for e in self.engs:
                stream = self.streams[e]
                extra = list(fin.items()) if e == "sp" else []
                if not stream and not extra:
                    continue

                def body(eng, stream=stream, extra=extra):
                    for waits, fn, key, inc in stream:
                        for k, v in waits:
                            eng.wait_ge(self.sems[k], v)
                        fn(eng).then_inc(self.sems[key], inc)
                    for k, v in extra:
                        eng.wait_ge(self.sems[k], v)

                getattr(block, handles[e])(body)


D = 1024
SEQ = 16384
NH = 8
DN = 128
DR = 64
DV = 128
QL = 384
KVL = 256
DFF = 2816
NFC = DFF // 128
ALPHA = float((2.0 * 2) ** 0.25)
LN_EPS = 1e-5
RMS_EPS = 1e-6
SCALE = float((DN + DR) ** -0.5)
TOK = 4096
MAGIC = 12582912.0
TWO_PI = float(2 * np.pi)


class Stage:
    def __init__(self, nc, tag):
        self.nc = nc
        self.tag = tag
        self.es = contextlib.ExitStack()
        self.s = Sched(nc, self.es, tag)
        self.n = 0

    def sb(self, shape, dt, name=None):
        self.n += 1
        t = self.es.enter_context(self.nc.sbuf_tensor("%s_%s%d" % (self.tag, name or "t", self.n), list(shape), dt))
        return t

    def ps(self, shape=(128, 512), dt=F32, name=None):
        self.n += 1
        t = self.es.enter_context(self.nc.psum_tensor("%s_%s%d" % (self.tag, name or "p", self.n), list(shape), dt))
        return t

    def finish(self):
        self.s.emit()
        self.es.close()


class RR:
    def __init__(self, tiles):
        self.t = [(t, Reg()) for t in tiles]
        self.i = 0

    def next(self):
        r = self.t[self.i % len(self.t)]
        self.i += 1
        return r


class Rot:
    def __init__(self, items):
        self.items = list(items)
        self.i = 0

    def next(self):
        r = self.items[self.i % len(self.items)]
        self.i += 1
        return r


def mm_group(s, out_ap, pairs, reads, writes):
    def fn(e):
        ins = None
        n = len(pairs)
        for i, (l, r) in enumerate(pairs):
            ins = e.matmul(out_ap, lhsT=l, rhs=r, start=(i == 0), stop=(i == n - 1))
        return ins
    return s.op("pe", fn, reads=reads, writes=writes)


def emit_layernorm(st, y, yR, W, out_tile, outR, g_sb, b_sb, ones_f, eps_sb, psA, psB, tmp):
    s = st.s
    ysq, ysqR = tmp["ysq"]
    s.op("act", lambda e: e.activation(out=ysq[:, :, :W], in_=y[:, :, :W], func=AF.Square), reads=[yR], writes=[ysqR])
    (mean_ps, meanR) = psA
    (ex2_ps, ex2R) = psB
    mm_group(s, mean_ps[:, :W], [(ones_f[:], y[:, c, :W]) for c in range(8)], [yR], [meanR])
    mm_group(s, ex2_ps[:, :W], [(ones_f[:], ysq[:, c, :W]) for c in range(8)], [ysqR], [ex2R])
    msq, msqR = tmp["msq"]
    rstd, rstdR = tmp["rstd"]
    mean_sb, mean_sbR = tmp["mean"]
    s.op("act", lambda e: e.activation(out=msq[:, :W], in_=mean_ps[:, :W], func=AF.Square), reads=[meanR], writes=[msqR])
    s.op("act", lambda e: e.activation(out=mean_sb[:, :W], in_=mean_ps[:, :W], func=AF.Copy), reads=[meanR], writes=[mean_sbR])
    s.op("dve", lambda e: e.tensor_tensor(out=rstd[:, :W], in0=ex2_ps[:, :W], in1=msq[:, :W], op=ALU.subtract), reads=[ex2R, msqR], writes=[rstdR])
    s.op("act", lambda e: e.activation(out=rstd[:, :W], in_=rstd[:, :W], func=AF.Sqrt, bias=eps_sb[:], scale=1.0), reads=[rstdR], writes=[rstdR])
    s.op("dve", lambda e: e.reciprocal(out=rstd[:, :W], in_=rstd[:, :W]), reads=[rstdR], writes=[rstdR])
    for c in range(8):
        eng = "dve" if c % 2 == 0 else "pool"
        s.op(eng, lambda e, c=c: e.tensor_tensor(out=ysq[:, c, :W], in0=y[:, c, :W], in1=mean_sb[:, :W], op=ALU.subtract), reads=[yR, mean_sbR], writes=[ysqR])
        s.op(eng, lambda e, c=c: e.tensor_tensor(out=ysq[:, c, :W], in0=ysq[:, c, :W], in1=rstd[:, :W], op=ALU.mult), reads=[ysqR, rstdR], writes=[ysqR])
        s.op("act", lambda e, c=c: e.activation(out=out_tile[:, c, :W], in_=ysq[:, c, :W], func=AF.Identity, bias=b_sb[:, c:c + 1], scale=g_sb[:, c:c + 1]), reads=[ysqR], writes=[outR])


def ln_tmp(st, W=512):
    return {
        "ysq": (st.sb([128, 8, W], F32, "ysq"), Reg()),
        "msq": (st.sb([128, W], F32, "msq"), Reg()),
        "rstd": (st.sb([128, W], F32, "rstd"), Reg()),
        "mean": (st.sb([128, W], F32, "mean"), Reg()),
    }


def load_vec_cols(st, dram_vec, nchunks, name, q="sp"):
    t = st.sb([128, nchunks], F32, name)
    r = Reg()
    st.s.dma(q, t[:], dram_vec.rearrange("(c p) -> p c", p=128), writes=[r], allow_slow_non_contiguous=True)
    return t, r


def const_tile(st, shape, dt, val, name, eng="dve"):
    t = st.sb(shape, dt, name)
    r = Reg()
    st.s.op(eng, lambda e: e.memset(t[:], val), writes=[r])
    return t, r


def stage_proj_res_ln(nc, tag, actT, w, residT, g_vec, b_vec, outT, tiles, mask=None, mask_cols=None):
    st = Stage(nc, tag)
    s = st.s
    wo = st.sb([128, 8, 1024], BF16, "wo")
    woR = Reg()
    wv = w.rearrange("(c p) n -> p c n", p=128)
    for c in range(8):
        s.dma("pool", wo[:, c, :], wv[:, c, :], writes=[woR])
    g_sb, gR = load_vec_cols(st, g_vec, 8, "g")
    b_sb, bR = load_vec_cols(st, b_vec, 8, "b")
    ones_f, onesR = const_tile(st, [128, 128], F32, 1.0 / 1024, "ones")
    eps_sb, epsR = const_tile(st, [128, 1], F32, LN_EPS, "eps")
    if mask is not None:
        mk = st.sb([128, 2], F32, "mask")
        mkR = Reg()
        s.dma("sp", mk[:], mask, writes=[mkR])
    acts = RR([st.sb([128, 8, 512], BF16, "act") for _ in range(2)])
    ress = RR([st.sb([128, 8, 512], F32, "res") for _ in range(2)])
    y = st.sb([128, 8, 512], F32, "y")
    yR = Reg()
    pp = RR([st.ps() for _ in range(3)])
    psA = (st.ps(), Reg())
    psB = (st.ps(), Reg())
    tmp = ln_tmp(st)
    av = actT.rearrange("(c p) t -> p c t", p=128)
    rv = residT.rearrange("(c p) t -> p c t", p=128)
    ov = outT.rearrange("(c p) t -> p c t", p=128)
    for ti, tl_ in enumerate(tiles):
        ci, c0, W = tl_ if len(tl_) == 3 else (tl_[0], tl_[0], tl_[1])
        a, aR = acts.next()
        r, rR = ress.next()
        o, oR = tmp["ysq"]
        kw = {"allow_slow_non_contiguous": True} if W < 8 else {}
        s.dma("sp", a[:, :, :W], av[:, :, ci:ci + W], writes=[aR], **kw)
        s.dma("act", r[:, :, :W], rv[:, :, ci:ci + W], writes=[rR], **kw)
        for d in range(8):
            p, pR = pp.next()
            mm_group(s, p[:, :W], [(wo[:, k, d * 128:(d + 1) * 128], a[:, k, :W]) for k in range(8)], [woR, aR], [pR])
            s.op("dve", lambda e, d=d, p=p, r=r, W=W: e.scalar_tensor_tensor(out=y[:, d, :W], in0=r[:, d, :W], scalar=ALPHA, in1=p[:, :W], op0=ALU.mult, op1=ALU.add),
                 reads=[rR, pR], writes=[yR])
        emit_layernorm(st, y, yR, W, o, oR, g_sb, b_sb, ones_f, eps_sb, psA, psB, tmp)
        if mask is not None and ti in mask_cols:
            mc = mask_cols[ti]
            for c in range(8):
                s.op("dve", lambda e, c=c, o=o, mc=mc, W=W: e.tensor_tensor(out=o[:, c, :W], in0=o[:, c, :W], in1=mk[:, mc:mc + W], op=ALU.mult), reads=[oR, mkR], writes=[oR])
        s.dma("sp", ov[:, :, c0:c0 + W], o[:, :, :W], reads=[oR], final=True, **kw)
    st.finish()


def stage_ffn_prep(nc, tag, w_up, w_down, wup_s, wdn_s):
    st = Stage(nc, tag)
    s = st.s
    stage32 = RR([st.sb([128, 5632], F32, "w32") for _ in range(2)])
    stage16 = RR([st.sb([128, 5632], BF16, "w16") for _ in range(2)])
    for c in range(8):
        a, aR = stage32.next()
        b, bR = stage16.next()
        s.dma("sp", a[:], w_up[c * 128:(c + 1) * 128, :], writes=[aR])
        h = 2816
        s.op("dve", lambda e, a=a, b=b: e.tensor_copy(out=b[:, :h], in_=a[:, :h]), reads=[aR], writes=[bR])
        s.op("pool", lambda e, a=a, b=b: e.tensor_copy(out=b[:, h:], in_=a[:, h:]), reads=[aR], writes=[bR])
        for two in range(2):
            s.dma("act", wup_s[:, :, c, two, :].rearrange("f p j -> p f j"),
                  b[:, two * 2816:(two + 1) * 2816].rearrange("p (f j) -> p f j", j=128), reads=[bR], final=True)
    for f in range(NFC):
        a, aR = stage32.next()
        b, bR = stage16.next()
        s.dma("sp", a[:, :1024], w_down[f * 128:(f + 1) * 128, :], writes=[aR])
        s.op("dve" if f % 2 else "pool", lambda e, a=a, b=b: e.tensor_copy(out=b[:, :1024], in_=a[:, :1024]), reads=[aR], writes=[bR])
        s.dma("act", wdn_s[:, :, f, :].rearrange("d p j -> p d j"), b[:, :1024].rearrange("p (d j) -> p d j", j=128), reads=[bR], final=True)
    st.finish()


def stage_ffn(nc, tag, xl, wup_s, wdn_s, w_conv, g_vec, b_vec, outT, out_bf=None):
    st = Stage(nc, tag)
    s = st.s
    BLK = 1024
    NB = TOK // BLK
    NC3 = 342
    g_sb, gR = load_vec_cols(st, g_vec, 8, "g")
    b_sb, bR = load_vec_cols(st, b_vec, 8, "b")
    ones_f, onesR = const_tile(st, [128, 128], F32, 1.0 / 1024, "ones")
    eps_sb, epsR = const_tile(st, [128, 1], F32, LN_EPS, "eps")
    wc = st.sb([128, 3, 2 * NFC], F32, "wc")
    wcR = Reg()
    for k in range(3):
        s.dma("sp", wc[:, k, :], w_conv[k, :].rearrange("(c p) -> p c", p=128), writes=[wcR], allow_slow_non_contiguous=True)
    stg = RR([st.sb([128, BLK + 2], F32, "stg") for _ in range(2)])
    x16 = st.sb([128, 8, BLK + 2], BF16, "x16")
    x16R = Reg()
    U = st.sb([128, NFC, BLK], BF16, "U")
    UR = [Reg() for _ in range(NFC)]
    wups = RR([st.sb([128, 8, 2, 128], BF16, "wup") for _ in range(3)])
    wdns = RR([st.sb([128, NFC, 128], BF16, "wdn") for _ in range(2)])
    hsb = [RR([st.sb([128, BLK + 2], F32, "h%d" % i) for _ in range(2)]) for i in range(2)]
    cv = [RR([st.sb([128, BLK], F32, "cv%d" % i) for _ in range(1)]) for i in range(2)]
    ptmp = st.sb([128, BLK], F32, "ptmp")
    ptmpR = Reg()
    hps = [[(st.ps(), Reg()) for _ in range(3)] for _ in range(2)]
    dps = RR([st.ps() for _ in range(2)])
    ress = RR([st.sb([128, 512], F32, "res") for _ in range(2)])
    y = st.sb([128, 8, 512], F32, "y")
    yR = Reg()
    tmp = ln_tmp(st)
    obf = st.sb([128, 8, 512], BF16, "obf") if out_bf is not None else None
    obfR = Reg()
    xv = xl.rearrange("(c p) t -> p c t", p=128)
    ov = outT.rearrange("(c p) t -> p c t", p=128)
    for blk in range(NB):
        t0 = blk * BLK
        for c in range(8):
            sg, sgR = stg.next()
            s.dma("sp", sg[:], xv[:, c, t0:t0 + BLK + 2], writes=[sgR])
            s.op("pool" if c % 2 else "dve", lambda e, c=c, sg=sg: e.tensor_copy(out=x16[:, c, :], in_=sg[:]), reads=[sgR], writes=[x16R])
        for f in range(NFC):
            wu, wuR = wups.next()
            s.dma("sp" if f % 2 else "act", wu[:].rearrange("p c t j -> p (c t j)"), wup_s[f].rearrange("p c t j -> p (c t j)"), writes=[wuR])
            cvt = []
            for ag in range(2):
                ch = f + ag * NFC
                for i in range(3):
                    p, pR = hps[ag][i]
                    mm_group(s, p[:, :NC3], [(wu[:, k, ag, :], x16[:, k, i * NC3:(i + 1) * NC3]) for k in range(8)], [wuR, x16R], [pR])
                h, hR = hsb[ag].next()
                for i in range(3):
                    p, pR = hps[ag][i]
                    s.op("act", lambda e, h=h, p=p, i=i: e.activation(out=h[:, i * NC3:(i + 1) * NC3], in_=p[:, :NC3], func=AF.Copy), reads=[pR], writes=[hR])
                c_, cR = cv[ag].next()
                if ag == 0:
                    s.op("dve", lambda e, c_=c_, h=h, ch=ch: e.tensor_scalar(out=c_[:], in0=h[:, 0:BLK], scalar1=wc[:, 0, ch:ch + 1], scalar2=None, op0=ALU.mult), reads=[hR, wcR], writes=[cR])
                    for k in (1, 2):
                        s.op("dve", lambda e, c_=c_, h=h, ch=ch, k=k: e.scalar_tensor_tensor(out=c_[:], in0=h[:, k:k + BLK], scalar=wc[:, k, ch:ch + 1], in1=c_[:], op0=ALU.mult, op1=ALU.add), reads=[hR, cR, wcR], writes=[cR])
                else:
                    s.op("pool", lambda e, c_=c_, h=h, ch=ch: e.tensor_scalar(out=c_[:], in0=h[:, 0:BLK], scalar1=wc[:, 0, ch:ch + 1], scalar2=None, op0=ALU.mult), reads=[hR, wcR], writes=[cR])
                    s.op("pool", lambda e, h=h, ch=ch: e.tensor_scalar(out=ptmp[:], in0=h[:, 1:1 + BLK], scalar1=wc[:, 1, ch:ch + 1], scalar2=None, op0=ALU.mult), reads=[hR, wcR], writes=[ptmpR])
                    s.op("pool", lambda e, c_=c_: e.tensor_tensor(out=c_[:], in0=c_[:], in1=ptmp[:], op=ALU.add), reads=[cR, ptmpR], writes=[cR])
                    s.op("dve", lambda e, c_=c_, h=h, ch=ch: e.scalar_tensor_tensor(out=c_[:], in0=h[:, 2:2 + BLK], scalar=wc[:, 2, ch:ch + 1], in1=c_[:], op0=ALU.mult, op1=ALU.add), reads=[hR, cR, wcR], writes=[cR])
                cvt.append((c_, cR))
            (ca, caR), (cg, cgR) = cvt
            s.op("act", lambda e, cg=cg: e.activation(out=cg[:], in_=cg[:], func=AF.Silu), reads=[cgR], writes=[cgR])
            s.op("dve", lambda e, ca=ca, cg=cg, f=f: e.tensor_tensor(out=U[:, f, :], in0=ca[:], in1=cg[:], op=ALU.mult), reads=[caR, cgR], writes=[UR[f]])
        for tt in range(BLK // 512):
            c0 = t0 + tt * 512
            for d in range(8):
                wd, wdR = wdns.next()
                s.dma("act", wd[:].rearrange("p f j -> p (f j)"), wdn_s[d].rearrange("p f j -> p (f j)"), writes=[wdR])
                r, rR = ress.next()
                s.dma("sp", r[:], xv[:, d, 1 + c0:1 + c0 + 512], writes=[rR])
                p, pR = dps.next()
                mm_group(s, p[:], [(wd[:, f, :], U[:, f, tt * 512:(tt + 1) * 512]) for f in range(NFC)], [wdR] + UR, [pR])
                s.op("dve", lambda e, d=d, p=p, r=r: e.scalar_tensor_tensor(out=y[:, d, :], in0=r[:], scalar=ALPHA, in1=p[:], op0=ALU.mult, op1=ALU.add),
                     reads=[rR, pR], writes=[yR])
            o, oR = tmp["ysq"]
            emit_layernorm(st, y, yR, 512, o, oR, g_sb, b_sb, ones_f, eps_sb, hps[0][0], hps[0][1], tmp)
            s.dma("sp", ov[:, :, c0:c0 + 512], o[:], reads=[oR], final=True)
            if out_bf is not None:
                s.op("pool", lambda e, o=o: e.tensor_copy(out=obf[:], in_=o[:]), reads=[oR], writes=[obfR])
                s.dma("act", out_bf.rearrange("(c p) t -> p c t", p=128)[:, :, c0:c0 + 512], obf[:], reads=[obfR], final=True)
    st.finish()


def rope_tables(st, pos_ap, t0, W, invf, invfR, posi, posiR, tabs, scale=None):
    s = st.s
    ang, angR = tabs["ang"]
    tmp, tmpR = tabs["tmp"]
    cos_t, cosR = tabs["cos"]
    sin_t, sinR = tabs["sin"]
    s.dma("act", posi[:, :W], pos_ap[t0:t0 + W].partition_broadcast(64), writes=[posiR])
    s.op("pool", lambda e: e.tensor_copy(out=ang[:, :W], in_=posi[:, :W]), reads=[posiR], writes=[angR])
    s.op("pool", lambda e: e.tensor_scalar(out=ang[:, :W], in0=ang[:, :W], scalar1=invf[:, 0:1], scalar2=None, op0=ALU.mult), reads=[angR, invfR], writes=[angR])
    for which, (dst, dstR) in enumerate(((sin_t, sinR), (cos_t, cosR))):
        if which == 1:
            s.op("pool", lambda e: e.tensor_scalar(out=ang[:, :W], in0=ang[:, :W], scalar1=float(np.pi / 2), scalar2=None, op0=ALU.add), reads=[angR], writes=[angR])
        s.op("pool", lambda e: e.tensor_scalar(out=tmp[:, :W], in0=ang[:, :W], scalar1=float(1.0 / TWO_PI), scalar2=MAGIC, op0=ALU.mult, op1=ALU.add), reads=[angR], writes=[tmpR])
        s.op("pool", lambda e: e.tensor_scalar(out=tmp[:, :W], in0=tmp[:, :W], scalar1=MAGIC, scalar2=-TWO_PI, op0=ALU.subtract, op1=ALU.mult), reads=[tmpR], writes=[tmpR])
        s.op("pool", lambda e: e.tensor_tensor(out=tmp[:, :W], in0=tmp[:, :W], in1=ang[:, :W], op=ALU.add), reads=[tmpR, angR], writes=[tmpR])
        s.op("act", lambda e, dst=dst: e.activation(out=dst[:, :W], in_=tmp[:, :W], func=AF.Sin), reads=[tmpR], writes=[dstR])
        if scale is not None:
            s.op("act", lambda e, dst=dst: e.activation(out=dst[:, :W], in_=dst[:, :W], func=AF.Copy, scale=scale), reads=[dstR], writes=[dstR])


def rope_tabs_alloc(st, W=512):
    return {k: (st.sb([64, W], F32, k), Reg()) for k in ("ang", "tmp", "cos", "sin")}


def apply_rope(st, src_ps, srcR, W, rot, rotR, rot_ps, rot_psR, tabs, work, dst, dstR):
    s = st.s
    (xs, xsR), (t1, t1R) = work
    cos_t, cosR = tabs["cos"]
    sin_t, sinR = tabs["sin"]
    s.op("act", lambda e: e.activation(out=xs[:, :W], in_=src_ps, func=AF.Copy), reads=[srcR], writes=[xsR])
    mm_group(s, rot_ps[0:64, :W], [(rot[:], xs[:, :W])], [rotR, xsR], [rot_psR])
    s.op("pool", lambda e: e.tensor_tensor(out=t1[:, :W], in0=xs[:, :W], in1=cos_t[:, :W], op=ALU.mult), reads=[xsR, cosR], writes=[t1R])
    s.op("dve", lambda e: e.tensor_tensor(out=xs[:, :W], in0=rot_ps[0:64, :W], in1=sin_t[:, :W], op=ALU.mult), reads=[rot_psR, sinR], writes=[xsR])
    s.op("dve", lambda e: e.tensor_tensor(out=dst, in0=xs[:, :W], in1=t1[:, :W], op=ALU.add), reads=[xsR, t1R], writes=[dstR])


def rms_scale(st, c_ps, nch, W, g_sb, gR, ones_f, eps_sb, stat, sq, rstd, dst, dstR):
    s = st.s
    (sq_t, sqR) = sq
    (stat_ps, statR) = stat
    (rs, rsR) = rstd
    for c in range(nch):
        p, pR = c_ps[c]
        s.op("act", lambda e, c=c, p=p: e.activation(out=sq_t[:, c, :W], in_=p[:, :W], func=AF.Square), reads=[pR], writes=[sqR])
    mm_group(s, stat_ps[:, :W], [(ones_f[:], sq_t[:, c, :W]) for c in range(nch)], [sqR], [statR])
    s.op("act", lambda e: e.activation(out=rs[:, :W], in_=stat_ps[:, :W], func=AF.Sqrt, bias=eps_sb[:], scale=1.0), reads=[statR], writes=[rsR])
    s.op("dve", lambda e: e.reciprocal(out=rs[:, :W], in_=rs[:, :W]), reads=[rsR], writes=[rsR])
    for c in range(nch):
        p, pR = c_ps[c]
        s.op("dve", lambda e, c=c, p=p: e.scalar_tensor_tensor(out=dst[:, c, :W], in0=p[:, :W], scalar=g_sb[:, c:c + 1], in1=rs[:, :W], op0=ALU.mult, op1=ALU.mult),
             reads=[pR, rsR, gR], writes=[dstR])


def stage_kv(nc, tag, xT, pos, w_in, g_kv, w_ukv, invf_c, rot_c, KT, KR, Vs):
    st = Stage(nc, tag)
    s = st.s
    win = st.sb([128, 8, 320], BF16, "win")
    winR = Reg()
    wiv = w_in.rearrange("(c p) n -> p c n", p=128)
    for c in range(8):
        s.dma("pool", win[:, c, :], wiv[:, c, 384:704], writes=[winR])
    wk = st.sb([128, 2, 8, 128], BF16, "wk")
    wv = st.sb([128, 2, 8, 128], BF16, "wv")
    wkR, wvR = Reg(), Reg()
    wuv = w_ukv.rearrange("(c p) (h t d) -> p c h t d", p=128, t=2, d=128)
    for c in range(2):
        s.dma("pool", wk[:, c], wuv[:, c, :, 0, :], writes=[wkR])
        s.dma("pool", wv[:, c], wuv[:, c, :, 1, :], writes=[wvR])
    g_sb, gR = load_vec_cols(st, g_kv, 2, "g")
    ones_f, _ = const_tile(st, [128, 128], F32, 1.0 / KVL, "ones")
    eps_sb, _ = const_tile(st, [128, 1], F32, RMS_EPS, "eps")
    invf = st.sb([64, 1], F32, "invf")
    invfR = Reg()
    s.dma("sp", invf[:], invf_c, writes=[invfR])
    rot = st.sb([64, 64], F32, "rot")
    rotR = Reg()
    s.dma("sp", rot[:], rot_c, writes=[rotR])
    posi = st.sb([64, 512], I32, "posi")
    posiR = Reg()
    tabs = rope_tabs_alloc(st)
    work = ((st.sb([64, 512], F32, "xs"), Reg()), (st.sb([64, 512], F32, "t1"), Reg()))
    x32s = RR([st.sb([128, 8, 512], F32, "x32") for _ in range(2)])
    x16s = RR([st.sb([128, 8, 512], BF16, "x16") for _ in range(2)])
    sq = (st.sb([128, 2, 512], F32, "sq"), Reg())
    rstd = (st.sb([128, 512], F32, "rstd"), Reg())
    ckvn = st.sb([128, 2, 512], BF16, "ckvn")
    ckvnR = Reg()
    kst = RR([st.sb([128, 8, 512], BF16, "kst") for _ in range(2)])
    vst = RR([st.sb([128, 4, 1024], BF16, "vst") for _ in range(2)])
    krb = RR([st.sb([64, 512], BF16, "krb") for _ in range(2)])
    cps = [(st.ps(), Reg()) for _ in range(3)]
    stat = (st.ps(), Reg())
    kps = RR([st.ps() for _ in range(2)])
    vps = RR([st.ps() for _ in range(2)])
    xv = xT.rearrange("(c p) t -> p c t", p=128)
    for ti in range(SEQ // 512):
        t0 = ti * 512
        x32, x32R = x32s.next()
        x16, x16R = x16s.next()
        s.dma("sp", x32[:], xv[:, :, t0:t0 + 512], writes=[x32R])
        for c in range(8):
            s.op("pool" if c % 2 else "dve", lambda e, c=c, x32=x32, x16=x16: e.tensor_copy(out=x16[:, c, :], in_=x32[:, c, :]), reads=[x32R], writes=[x16R])
        rope_tables(st, pos, t0, 512, invf, invfR, posi, posiR, tabs)
        for j in range(3):
            p, pR = cps[j]
            M = 128 if j < 2 else 64
            mm_group(s, p[0:M, :], [(win[:, k, j * 128:j * 128 + M], x16[:, k, :]) for k in range(8)], [winR, x16R], [pR])
        rms_scale(st, cps[:2], 2, 512, g_sb, gR, ones_f, eps_sb, stat, sq, rstd, ckvn, ckvnR)
        kb, kbR = krb.next()
        rp, rpR = kps.next()
        apply_rope(st, cps[2][0][0:64, :], cps[2][1], 512, rot, rotR, rp, rpR, tabs, work, kb[:], kbR)
        s.dma("sp", KR[:, t0:t0 + 512], kb[:], reads=[kbR], final=True)
        ks, ksR = kst.next()
        for h in range(NH):
            p, pR = kps.next()
            mm_group(s, p[:], [(wk[:, c, h, :], ckvn[:, c, :]) for c in range(2)], [wkR, ckvnR], [pR])
            if h % 2:
                s.op("act", lambda e, p=p, h=h, ks=ks: e.activation(out=ks[:, h, :], in_=p[:], func=AF.Copy), reads=[pR], writes=[ksR])
            else:
                s.op("dve", lambda e, p=p, h=h, ks=ks: e.tensor_copy(out=ks[:, h, :], in_=p[:]), reads=[pR], writes=[ksR])
        s.dma("sp", KT[:, :, t0:t0 + 512].rearrange("h p t -> p h t"), ks[:], reads=[ksR], final=True)
        vs, vsR = vst.next()
        for q in range(4):
            for half in range(2):
                p, pR = vps.next()
                mm_group(s, p[:], [(ckvn[:, c, q * 128:(q + 1) * 128], wv[:, c, half * 4:(half + 1) * 4, :].rearrange("p h d -> p (h d)")) for c in range(2)],
                         [wvR, ckvnR], [pR])
                if half:
                    s.op("act", lambda e, p=p, q=q, vs=vs: e.activation(out=vs[:, q, 512:1024], in_=p[:], func=AF.Copy), reads=[pR], writes=[vsR])
                else:
                    s.op("dve", lambda e, p=p, q=q, vs=vs: e.tensor_copy(out=vs[:, q, 0:512], in_=p[:]), reads=[pR], writes=[vsR])
        for q in range(4):
            s.dma("act", Vs[:, :, ti * 4 + q, :].rearrange("h p d -> p h d"), vs[:, q, :].rearrange("p (h d) -> p h d", d=128), reads=[vsR], final=True)
    st.finish()


def stage_q(nc, tag, xTq, posq, w_in, g_q, w_uq, invf_c, rot_c, QN, QR):
    st = Stage(nc, tag)
    s = st.s
    win = st.sb([128, 8, 384], BF16, "win")
    winR = Reg()
    wiv = w_in.rearrange("(c p) n -> p c n", p=128)
    for c in range(8):
        s.dma("pool", win[:, c, :], wiv[:, c, 0:384], writes=[winR])
    wqn = st.sb([128, 3, 8, 128], BF16, "wqn")
    wqr = st.sb([128, 3, 8, 64], BF16, "wqr")
    wqnR, wqrR = Reg(), Reg()
    wuv = w_uq.rearrange("(c p) (h d) -> p c h d", p=128, d=192)
    for c in range(3):
        s.dma("pool", wqn[:, c], wuv[:, c, :, 0:128], writes=[wqnR])
        s.dma("pool", wqr[:, c], wuv[:, c, :, 128:192], writes=[wqrR])
    g_sb, gR = load_vec_cols(st, g_q, 3, "g")
    ones_f, _ = const_tile(st, [128, 128], F32, 1.0 / QL, "ones")
    eps_sb, _ = const_tile(st, [128, 1], F32, RMS_EPS, "eps")
    invf = st.sb([64, 1], F32, "invf")
    invfR = Reg()
    s.dma("sp", invf[:], invf_c, writes=[invfR])
    rot = st.sb([64, 64], F32, "rot")
    rotR = Reg()
    s.dma("sp", rot[:], rot_c, writes=[rotR])
    posi = st.sb([64, 512], I32, "posi")
    posiR = Reg()
    tabs = rope_tabs_alloc(st)
    work = ((st.sb([64, 512], F32, "xs"), Reg()), (st.sb([64, 512], F32, "t1"), Reg()))
    x32s = RR([st.sb([128, 8, 512], F32, "x32") for _ in range(2)])
    x16s = RR([st.sb([128, 8, 512], BF16, "x16") for _ in range(2)])
    sq = (st.sb([128, 3, 512], F32, "sq"), Reg())
    rstd = (st.sb([128, 512], F32, "rstd"), Reg())
    cqn = st.sb([128, 3, 512], BF16, "cqn")
    cqnR = Reg()
    qst = RR([st.sb([128, 8, 512], BF16, "qst") for _ in range(2)])
    qrs = RR([st.sb([64, 8, 512], BF16, "qrs") for _ in range(2)])
    cps = [(st.ps(), Reg()) for _ in range(3)]
    stat = (st.ps(), Reg())
    qps = RR([st.ps() for _ in range(2)])
    rps = RR([st.ps() for _ in range(2)])
    xv = xTq.rearrange("(c p) t -> p c t", p=128)
    for ti in range(TOK // 512):
        t0 = ti * 512
        x32, x32R = x32s.next()
        x16, x16R = x16s.next()
        s.dma("sp", x32[:], xv[:, :, t0:t0 + 512], writes=[x32R])
        for c in range(8):
            s.op("pool" if c % 2 else "dve", lambda e, c=c, x32=x32, x16=x16: e.tensor_copy(out=x16[:, c, :], in_=x32[:, c, :]), reads=[x32R], writes=[x16R])
        rope_tables(st, posq, t0, 512, invf, invfR, posi, posiR, tabs, scale=SCALE)
        for j in range(3):
            p, pR = cps[j]
            mm_group(s, p[:], [(win[:, k, j * 128:(j + 1) * 128], x16[:, k, :]) for k in range(8)], [winR, x16R], [pR])
        rms_scale(st, cps, 3, 512, g_sb, gR, ones_f, eps_sb, stat, sq, rstd, cqn, cqnR)
        qs, qsR = qst.next()
        qr, qrR = qrs.next()
        for h in range(NH):
            p, pR = qps.next()
            mm_group(s, p[:], [(wqn[:, c, h, :], cqn[:, c, :]) for c in range(3)], [wqnR, cqnR], [pR])
            s.op("act", lambda e, p=p, h=h, qs=qs: e.activation(out=qs[:, h, :], in_=p[:], func=AF.Copy, scale=SCALE), reads=[pR], writes=[qsR])
            p2, p2R = qps.next()
            mm_group(s, p2[0:64, :], [(wqr[:, c, h, :], cqn[:, c, :]) for c in range(3)], [wqrR, cqnR], [p2R])
            rp, rpR = rps.next()
            apply_rope(st, p2[0:64, :], p2R, 512, rot, rotR, rp, rpR, tabs, work, qr[:, h, :], qrR)
        s.dma("sp", QN[:, :, t0:t0 + 512].rearrange("h p t -> p h t"), qs[:], reads=[qsR], final=True)
        s.dma("sp", QR[:, :, t0:t0 + 512].rearrange("h p t -> p h t"), qr[:], reads=[qrR], final=True)
    st.finish()


def stage_attn(nc, tag, KT, KR, Vs, QN, QR, OT):
    st = Stage(nc, tag)
    s = st.s
    NQ = TOK // 512
    NK = SEQ // 128
    kr = st.sb([64, SEQ], BF16, "kr")
    krR = [Reg() for _ in range(4)]
    for j in range(4):
        s.dma("sp", kr[:, j * 4096:(j + 1) * 4096], KR[:, j * 4096:(j + 1) * 4096], writes=[krR[j]])
    kt = st.sb([128, SEQ], BF16, "kt")
    ktR = [Reg() for _ in range(4)]
    vv = st.sb([128, NK, 128], BF16, "vv")
    vvR = [Reg() for _ in range(4)]
    qns = RR([st.sb([128, TOK], BF16, "qn") for _ in range(2)])
    qrs = RR([st.sb([64, TOK], BF16, "qr") for _ in range(2)])
    ones_b, onesR = const_tile(st, [128, 128], BF16, 1.0, "ones")
    pts = RR([st.sb([128, 512], BF16, "pt") for _ in range(3)])
    sps = RR([st.ps() for _ in range(3)])
    ops_ = RR([st.ps() for _ in range(2)])
    dps = RR([st.ps() for _ in range(2)])
    rden = st.sb([128, 512], F32, "rden")
    rdenR = Reg()
    osb = RR([st.sb([128, 512], BF16, "osb") for _ in range(2)])
    pend = []

    def flush_one():
        fn = pend.pop(0)
        fn()

    for h in range(NH):
        for j in range(4):
            s.dma("sp", kt[:, j * 4096:(j + 1) * 4096], KT[h, :, j * 4096:(j + 1) * 4096], writes=[ktR[j]])
        for j in range(4):
            s.dma("sp", vv[:, j * 32:(j + 1) * 32, :], Vs[h, :, j * 32:(j + 1) * 32, :], writes=[vvR[j]])
        qn, qnR = qns.next()
        qr, qrR = qrs.next()
        s.dma("pool", qn[:], QN[h], writes=[qnR])
        s.dma("pool", qr[:], QR[h], writes=[qrR])
        for qt in range(NQ):
            o_ps, oR = ops_.next()
            d_ps, dR = dps.next()
            qsl = slice(qt * 512, (qt + 1) * 512)
            for k in range(NK):
                j = k // 32
                sp_, spR = sps.next()
                pt, ptR = pts.next()
                ksl = slice(k * 128, (k + 1) * 128)
                mm_group(s, sp_[:], [(kt[:, ksl], qn[:, qsl]), (kr[:, ksl], qr[:, qsl])], [ktR[j], krR[j], qnR, qrR], [spR])
                s.op("act", lambda e, sp_=sp_, pt=pt: e.activation(out=pt[:], in_=sp_[:], func=AF.Exp), reads=[spR], writes=[ptR])

                def pv(k=k, j=j, pt=pt, ptR=ptR, o_ps=o_ps, oR=oR, d_ps=d_ps, dR=dR, h=h, qt=qt, qsl=qsl):
                    def fn(e):
                        e.matmul(o_ps[:], lhsT=vv[:, k, :], rhs=pt[:], start=(k == 0), stop=(k == NK - 1))
                        return e.matmul(d_ps[:], lhsT=ones_b[:], rhs=pt[:], start=(k == 0), stop=(k == NK - 1))
                    s.op("pe", fn, reads=[vvR[j], ptR, onesR], writes=[oR, dR])
                    if k == NK - 1:
                        ob, obR = osb.next()
                        s.op("dve", lambda e: e.reciprocal(out=rden[:], in_=d_ps[:]), reads=[dR], writes=[rdenR])
                        s.op("dve", lambda e: e.tensor_tensor(out=ob[:], in0=o_ps[:], in1=rden[:], op=ALU.mult), reads=[oR, rdenR], writes=[obR])
                        s.dma("pool", OT[h, :, qsl], ob[:], reads=[obR], final=True)
                pend.append(pv)
                if len(pend) > 2:
                    flush_one()
    while pend:
        flush_one()
    st.finish()


NFFT = 2 * SEQ
CB = 8
NCB = 128 // CB


def hyena_consts(ch0):
    L = SEQ
    c = {}
    pp = np.arange(128)[:, None].astype(np.float64)
    b256 = np.arange(256)[None, :].astype(np.float64)
    th = 2 * np.pi * pp * b256 / 256
    c["F1r"] = np.concatenate([np.cos(th), -np.sin(th)], 1)
    c["F1i"] = np.concatenate([np.sin(th), np.cos(th)], 1)
    th2 = 2 * np.pi * (pp + 128) * b256 / 256
    c["F1h"] = np.concatenate([np.cos(th2), -np.sin(th2)], 1)
    a128 = np.arange(128)[None, :].astype(np.float64)
    ps = 2 * np.pi * pp * a128 / 128
    c["C3"] = np.cos(ps)
    c["S3"] = np.sin(ps)
    c["nS3"] = -np.sin(ps)
    ph = 2 * np.pi * pp * b256 / NFFT
    c["TwA"] = np.concatenate([np.cos(ph), -np.sin(ph)], 1)
    c["TwB"] = np.concatenate([-np.sin(ph), np.cos(ph)], 1)
    c["I1r"] = np.concatenate([np.cos(ps), np.sin(ps)], 1)
    c["I1i"] = np.concatenate([-np.sin(ps), np.cos(ps)], 1)
    itA = np.zeros((128, 2, 2, 256))
    itB = np.zeros((128, 2, 2, 256))
    f128 = np.arange(128)[None, :].astype(np.float64)
    for j in range(2):
        phi = 2 * np.pi * (pp + 128 * j) * f128 / NFFT
        for cc in range(2):
            itA[:, j, cc, :128] = np.cos(phi)
            itA[:, j, cc, 128:] = np.sin(phi)
            itB[:, j, cc, :128] = np.sin(phi)
            itB[:, j, cc, 128:] = np.cos(phi)
    c["ItA"] = itA.reshape(128, 1024)
    c["ItB"] = itB.reshape(128, 1024)
    i2 = np.zeros((128, 6, 128))
    for j in range(2):
        om = 2 * np.pi * (pp + 128 * j) * a128 / 256
        i2[:, j * 3 + 0] = np.cos(om)
        i2[:, j * 3 + 1] = np.sin(om)
        i2[:, j * 3 + 2] = -np.sin(om)
    c["I2"] = i2.reshape(128, 768)
    c["ident"] = np.eye(128)
    def feats(m):
        m = m.astype(np.float64)
        t = (m / (L - 1)).astype(np.float32)
        wv = (2.0 * np.pi * m.astype(np.float32) / np.float32(L)).astype(np.float32)
        fr = np.linspace(1e-4, 15, 16, dtype=np.float32)
        z = np.concatenate([t[None, :], np.cos(fr[:, None] * wv[None, :]), -np.sin(fr[:, None] * wv[None, :])], 0)
        return z.astype(np.float32), t
    mf = np.arange(L)
    n = np.arange(L, 2 * L)
    mb = 2 * L - n
    mb[0] = 0
    c["zf"], tf = feats(mf)
    c["zb"], tb = feats(mb)
    c["tlf"] = tf.reshape(128, 128)
    c["tlb"] = tb.reshape(128, 128)
    deltas = np.abs(np.linspace(np.log(0.3) / 1e-2, np.log(1.5) / 1e-2, D, dtype=np.float32))
    c["ndelta"] = np.broadcast_to(-deltas[ch0:ch0 + 128][None, :], (128, 128))
    return {k: np.ascontiguousarray(v, dtype=np.float32) for k, v in c.items()}


HY_CONST_SHAPES = {"F1r": [128, 512], "F1i": [128, 512], "F1h": [128, 512], "C3": [128, 128], "S3": [128, 128],
                   "nS3": [128, 128], "TwA": [128, 512], "TwB": [128, 512], "I1r": [128, 256], "I1i": [128, 256],
                   "ItA": [128, 1024], "ItB": [128, 1024], "I2": [128, 768], "ident": [128, 128],
                   "zf": [33, SEQ], "zb": [33, SEQ], "tlf": [128, 128], "tlb": [128, 128], "ndelta": [128, 128]}


def stage_hy_proj(nc, tag, x1b, w_in_c, wsh_c, UC):
    st = Stage(nc, tag)
    s = st.s
    win = st.sb([128, 8, 3, 128], BF16, "win")
    winR = Reg()
    wv = w_in_c.rearrange("(c p) j n -> p c j n", p=128)
    for c in range(8):
        s.dma("pool", win[:, c], wv[:, c], writes=[winR])
    wsh = st.sb([128, 9], F32, "wsh")
    wshR = Reg()
    s.dma("sp", wsh[:], wsh_c, writes=[wshR])
    xs = RR([st.sb([128, 8, 514], BF16, "x") for _ in range(2)])
    us = RR([st.sb([128, 514], F32, "u") for _ in range(2)])
    ucs = RR([st.sb([128, 512], F32, "uc") for _ in range(3)])
    pps = RR([st.ps() for _ in range(6)])
    xv = x1b.rearrange("(c p) t -> p c t", p=128)
    for b in range(2):
        for ti in range(SEQ // 512):
            t0 = ti * 512
            x, xR = xs.next()
            lo = 1 if ti == 0 else 0
            hi = 513 if ti == SEQ // 512 - 1 else 514
            if lo:
                s.op("pool", lambda e, x=x: e.memset(x[:, :, 0:1], 0.0), writes=[xR])
            if hi == 513:
                s.op("pool", lambda e, x=x: e.memset(x[:, :, 513:514], 0.0), writes=[xR])
            s.dma("sp", x[:, :, lo:hi], xv[:, :, b * SEQ + t0 - 1 + lo:b * SEQ + t0 - 1 + hi], writes=[xR])
            for j in range(3):
                u, uR = us.next()
                for half in range(2):
                    p, pR = pps.next()
                    mm_group(s, p[:, :257], [(win[:, k, j, :], x[:, k, half * 257:(half + 1) * 257]) for k in range(8)], [winR, xR], [pR])
                    s.op("act", lambda e, u=u, p=p, half=half: e.activation(out=u[:, half * 257:(half + 1) * 257], in_=p[:, :257], func=AF.Copy), reads=[pR], writes=[uR])
                uc, ucR = ucs.next()
                s.op("dve", lambda e, uc=uc, u=u, j=j: e.tensor_scalar(out=uc[:], in0=u[:, 0:512], scalar1=wsh[:, j * 3:j * 3 + 1], scalar2=None, op0=ALU.mult), reads=[uR, wshR], writes=[ucR])
                for k in (1, 2):
                    s.op("dve", lambda e, uc=uc, u=u, j=j, k=k: e.scalar_tensor_tensor(out=uc[:], in0=u[:, k:k + 512], scalar=wsh[:, j * 3 + k:j * 3 + k + 1], in1=uc[:], op0=ALU.mult, op1=ALU.add), reads=[uR, ucR, wshR], writes=[ucR])
                s.dma("pool", UC[j, b, :, t0:t0 + 512], uc[:], reads=[ucR], final=True)
    st.finish()


def stage_hy_transpose(nc, tag, UC, ident_c, VG):
    st = Stage(nc, tag)
    s = st.s
    ident = st.sb([128, 128], F32, "ident")
    idR = Reg()
    s.dma("sp", ident[:], ident_c, writes=[idR])
    uct = st.sb([128, SEQ], F32, "uct")
    uctR = Reg()
    vgt = st.sb([128, 128, 128], F32, "vgt")
    vgtR = Reg()
    pps = RR([st.ps() for _ in range(4)])
    for j in range(3):
        for b in range(2):
            for q in range(4):
                s.dma("sp" if q % 2 else "act", uct[:, q * 4096:(q + 1) * 4096], UC[j, b, :, q * 4096:(q + 1) * 4096], writes=[uctR])
            for f0 in range(0, 128, 4):
                p, pR = pps.next()

                def fn(e, p=p, f0=f0):
                    ins = None
                    for q in range(4):
                        ins = e.transpose(p[:, q * 128:(q + 1) * 128], uct[:, f0 + q::128], ident[:])
                    return ins
                s.op("pe", fn, reads=[uctR, idR], writes=[pR])
                eng = "dve" if (f0 // 4) % 2 else "act"
                dst = vgt[:, :, f0:f0 + 4].rearrange("p c f -> p f c")
                src = p[:, :].rearrange("p (f c) -> p f c", c=128)
                if eng == "dve":
                    s.op("dve", lambda e, dst=dst, src=src: e.tensor_copy(out=dst, in_=src), reads=[pR], writes=[vgtR])
                else:
                    s.op("act", lambda e, dst=dst, src=src: e.activation(out=dst, in_=src, func=AF.Copy), reads=[pR], writes=[vgtR])
            for q in range(4):
                s.dma("sp" if q % 2 else "act", VG[j, b, :, q * 32:(q + 1) * 32, :], vgt[:, q * 32:(q + 1) * 32, :], reads=[vgtR], final=True)
    st.finish()


def load_fft_consts(st, hc, names_bf=(), names_f32=()):
    out = {}
    for nme in names_bf:
        t = st.sb(HY_CONST_SHAPES[nme], BF16, nme)
        r = Reg()
        st.s.dma("pool", t[:], hc[nme], writes=[r])
        out[nme] = (t, r)
    for nme in names_f32:
        t = st.sb(HY_CONST_SHAPES[nme], F32, nme)
        r = Reg()
        st.s.dma("sp", t[:], hc[nme], writes=[r])
        out[nme] = (t, r)
    return out


def fft_forward(st, cst, srcs, Yt, pbanks, xbanks, mwork, consume):
    s = st.s
    (Ytr, YtrR), (Yti, YtiR) = Yt
    TwA, TwAR = cst["TwA"]
    TwB, TwBR = cst["TwB"]
    for c in range(CB):
        p, pR = pbanks.next()
        mm_group(s, p[:], [(t[:, c, :], cst[nm][0][:]) for (t, tR, nm) in srcs], [tR for (t, tR, nm) in srcs] + [cst[nm][1] for (_, _, nm) in srcs], [pR])
        (mA, mAR), (mB, mBR) = mwork.next()
        s.op("dve", lambda e, p=p, mA=mA: e.tensor_tensor(out=mA[:], in0=p[:], in1=TwA[:], op=ALU.mult), reads=[pR, TwAR], writes=[mAR])
        s.op("dve", lambda e, p=p, mB=mB: e.tensor_tensor(out=mB[:], in0=p[:], in1=TwB[:], op=ALU.mult), reads=[pR, TwBR], writes=[mBR])
        s.op("pool", lambda e, mA=mA, c=c: e.tensor_tensor(out=Ytr[:, c, :], in0=mA[:, 0:256], in1=mA[:, 256:512], op=ALU.subtract), reads=[mAR], writes=[YtrR])
        s.op("pool", lambda e, mB=mB, c=c: e.tensor_tensor(out=Yti[:, c, :], in0=mB[:, 0:256], in1=mB[:, 256:512], op=ALU.add), reads=[mBR], writes=[YtiR])
    C3, C3R = cst["C3"]
    S3, S3R = cst["S3"]
    nS3, nS3R = cst["nS3"]
    for g in range(CB // 2):
        yr = Ytr[:, 2 * g:2 * g + 2, :].rearrange("p c b -> p (c b)")
        yi = Yti[:, 2 * g:2 * g + 2, :].rearrange("p c b -> p (c b)")
        xr, xrR = xbanks.next()
        xi, xiR = xbanks.next()
        mm_group(s, xr[:], [(C3[:], yr), (S3[:], yi)], [C3R, S3R, YtrR, YtiR], [xrR])
        mm_group(s, xi[:], [(C3[:], yi), (nS3[:], yr)], [C3R, nS3R, YtrR, YtiR], [xiR])
        consume(g, xr, xrR, xi, xiR)


def cmul_to(s, xr, xrR, xi, xiR, kr, ki, kR, m4, outr, outi, outR):
    (m1, m1R), (m2, m2R), (m3, m3R), (m4_, m4R) = m4
    s.op("dve", lambda e: e.tensor_tensor(out=m1[:], in0=xr[:], in1=kr, op=ALU.mult), reads=[xrR, kR], writes=[m1R])
    s.op("dve", lambda e: e.tensor_tensor(out=m2[:], in0=xi[:], in1=ki, op=ALU.mult), reads=[xiR, kR], writes=[m2R])
    s.op("dve", lambda e: e.tensor_tensor(out=m3[:], in0=xr[:], in1=ki, op=ALU.mult), reads=[xrR, kR], writes=[m3R])
    s.op("dve", lambda e: e.tensor_tensor(out=m4_[:], in0=xi[:], in1=kr, op=ALU.mult), reads=[xiR, kR], writes=[m4R])
    s.op("pool", lambda e: e.tensor_tensor(out=outr, in0=m1[:], in1=m2[:], op=ALU.subtract), reads=[m1R, m2R], writes=[outR])
    s.op("pool", lambda e: e.tensor_tensor(out=outi, in0=m3[:], in1=m4_[:], op=ALU.add), reads=[m3R, m4R], writes=[outR])


def stage_hy_filter(nc, tag, hc, fw, fwout_c, Kf, H3):
    st = Stage(nc, tag + "m")
    s = st.s
    w1 = st.sb([33, 64], F32, "w1")
    w2 = st.sb([64, 64], F32, "w2")
    w3 = st.sb([64, 64], F32, "w3")
    wR = Reg()
    s.dma("sp", w1[:], fw["fw1"], writes=[wR])
    s.dma("sp", w2[:], fw["fw2"], writes=[wR])
    s.dma("sp", w3[:], fw["fw3"], writes=[wR])
    fr = st.sb([64, 1], F32, "fr")
    fbs = st.sb([64, 3], F32, "fb")
    frR = Reg()
    s.dma("sp", fr[:], fw["freq"], writes=[frR])
    for i, k in enumerate(("fb1", "fb2", "fb3")):
        s.dma("sp", fbs[:, i:i + 1], fw[k], writes=[frR])
    s.op("dve", lambda e: e.tensor_tensor(out=fbs[:], in0=fbs[:], in1=fr[:, 0:1].to_broadcast([64, 3]), op=ALU.mult), reads=[frR], writes=[frR])
    zts = RR([st.sb([33, 512], F32, "z") for _ in range(2)])
    hs = RR([st.sb([64, 512], F32, "h") for _ in range(3)])
    ts_ = RR([st.sb([64, 512], F32, "t") for _ in range(2)])
    pps = RR([st.ps() for _ in range(4)])
    for d, zn in enumerate(("zf", "zb")):
        for ti in range(SEQ // 512):
            z, zR = zts.next()
            s.dma("sp", z[:], hc[zn][:, ti * 512:(ti + 1) * 512], writes=[zR])
            cur, curR = z, zR
            for li, wt in enumerate((w1, w2, w3)):
                p, pR = pps.next()
                mm_group(s, p[0:64, :], [(wt[:], cur[:])], [wR, curR], [pR])
                a, aR = hs.next()
                t, tR = ts_.next()
                s.op("act", lambda e, a=a, p=p, li=li: e.activation(out=a[:], in_=p[0:64, :], func=AF.Identity, bias=fbs[:, li:li + 1], scale=fr[:, 0:1]), reads=[pR, frR], writes=[aR])
                s.op("dve", lambda e, a=a, t=t: e.tensor_scalar(out=t[:], in0=a[:], scalar1=float(1.0 / TWO_PI), scalar2=MAGIC, op0=ALU.mult, op1=ALU.add), reads=[aR], writes=[tR])
                s.op("dve", lambda e, t=t: e.tensor_scalar(out=t[:], in0=t[:], scalar1=MAGIC, scalar2=-TWO_PI, op0=ALU.subtract, op1=ALU.mult), reads=[tR], writes=[tR])
                s.op("dve", lambda e, a=a, t=t: e.tensor_tensor(out=t[:], in0=t[:], in1=a[:], op=ALU.add), reads=[tR, aR], writes=[tR])
                s.op("act", lambda e, a=a, t=t: e.activation(out=a[:], in_=t[:], func=AF.Sin), reads=[tR], writes=[aR])
                cur, curR = a, aR
            s.dma("sp", H3[d, :, ti * 512:(ti + 1) * 512], cur[:], reads=[curR], final=True)
    st.finish()
    st = Stage(nc, tag + "k")
    s = st.s
    cst = load_fft_consts(st, hc, names_bf=("F1r", "F1h", "C3", "S3", "nS3"), names_f32=("TwA", "TwB", "tlf", "tlb", "ndelta"))
    fwo = st.sb([64, 4, 128], F32, "fwo")
    fwoR = Reg()
    s.dma("sp", fwo[:], fwout_c, writes=[fwoR])
    h3 = st.sb([64, SEQ], F32, "h3")
    h3R = Reg()
    k2 = [(st.sb([128, 128, 128], BF16, "k2%d" % d), Reg()) for d in range(2)]
    dec = RR([st.sb([128, 4, 128], F32, "dec") for _ in range(2)])
    kps = RR([st.ps() for _ in range(2)])
    pbanks = RR([st.ps() for _ in range(2)])
    xbanks = RR([st.ps() for _ in range(4)])
    Yt = ((st.sb([128, CB, 256], BF16, "ytr"), Reg()), (st.sb([128, CB, 256], BF16, "yti"), Reg()))
    mwork = Rot([((st.sb([128, 512], F32, "mA"), Reg()), (st.sb([128, 512], F32, "mB"), Reg())) for _ in range(2)])
    kst = RR([st.sb([128, 2, 512], BF16, "kst") for _ in range(3)])
    ndl, ndlR = cst["ndelta"]
    for o in range(2):
        for d in range(2):
            for q in range(4):
                s.dma("sp" if q % 2 else "act", h3[:, q * 4096:(q + 1) * 4096], H3[d, :, q * 4096:(q + 1) * 4096], writes=[h3R])
            kt, ktR = k2[d]
            tl, tlR = cst["tlf" if d == 0 else "tlb"]
            for f0 in range(0, 128, 4):
                p, pR = kps.next()

                def fn(e, p=p, f0=f0, o=o, d=d):
                    ins = None
                    for q in range(4):
                        ins = e.matmul(p[:, q * 128:(q + 1) * 128], lhsT=h3[:, f0 + q::128], rhs=fwo[:, o * 2 + d, :], start=True, stop=True)
                    return ins
                s.op("pe", fn, reads=[h3R, fwoR], writes=[pR])
                dc, dcR = dec.next()
                s.op("pool", lambda e, dc=dc, tl=tl, f0=f0: e.tensor_tensor(out=dc[:], in0=tl[:, f0:f0 + 4].unsqueeze(2).to_broadcast([128, 4, 128]),
                                                                      in1=ndl[:, :].unsqueeze(1).to_broadcast([128, 4, 128]), op=ALU.mult), reads=[tlR, ndlR], writes=[dcR])
                s.op("act", lambda e, dc=dc: e.activation(out=dc[:], in_=dc[:], func=AF.Exp), reads=[dcR], writes=[dcR])
                s.op("dve", lambda e, dc=dc, p=p, kt=kt, f0=f0: e.tensor_tensor(out=kt[:, :, f0:f0 + 4].rearrange("p c f -> p f c"), in0=p[:, :].rearrange("p (f c) -> p f c", c=128), in1=dc[:], op=ALU.mult),
                     reads=[pR, dcR], writes=[ktR])
        (kf_, kfR), (kb_, kbR) = k2
        s.op("dve", lambda e: e.tensor_tensor(out=kf_[0:1, :, 0:1], in0=kf_[0:1, :, 0:1], in1=kb_[0:1, :, 0:1], op=ALU.add), reads=[kfR, kbR], writes=[kfR])
        s.op("dve", lambda e: e.memset(kb_[0:1, :, 0:1], 0.0), reads=[kfR], writes=[kbR])
        for cb in range(NCB):
            srcs = [(kf_[:, cb * CB:(cb + 1) * CB, :], kfR, "F1r"), (kb_[:, cb * CB:(cb + 1) * CB, :], kbR, "F1h")]

            def consume(g, xr, xrR, xi, xiR, o=o, cb=cb):
                ks, ksR = kst.next()
                s.op("act", lambda e: e.activation(out=ks[:, 0, :], in_=xr[:], func=AF.Copy, scale=1.0 / NFFT), reads=[xrR], writes=[ksR])
                s.op("act", lambda e: e.activation(out=ks[:, 1, :], in_=xi[:], func=AF.Copy, scale=1.0 / NFFT), reads=[xiR], writes=[ksR])
                c0 = cb * CB + 2 * g
                s.dma("sp", Kf[o, :, :, c0:c0 + 2, :].rearrange("r a c b -> a r (c b)"), ks[:], reads=[ksR], final=True)
            fft_forward(st, cst, srcs, Yt, pbanks, xbanks, mwork, consume)
    st.finish()


def stage_hy_conv(nc, tag, hc, VG, Kf, dbias_c, z2T):
    st = Stage(nc, tag)
    s = st.s
    cst = load_fft_consts(st, hc, names_bf=("F1r", "F1i", "C3", "S3", "nS3", "I1r", "I1i", "I2", "ident"), names_f32=("TwA", "TwB", "ItA", "ItB"))
    db = st.sb([128, 2, 128], F32, "db")
    dbR = Reg()
    for o in range(2):
        s.dma("sp", db[:, o, :], dbias_c[o, :].partition_broadcast(128), writes=[dbR])
    ident, identR = cst["ident"]
    I1r, I1rR = cst["I1r"]
    I1i, I1iR = cst["I1i"]
    I2, I2R = cst["I2"]
    ItA, ItAR = cst["ItA"]
    ItB, ItBR = cst["ItB"]
    v32 = [(st.sb([128, CB, 128], F32, "v32_%d" % b), Reg()) for b in range(2)]
    g32 = [(st.sb([128, CB, 128], F32, "g32_%d" % b), Reg()) for b in range(2)]
    xin = [(st.sb([128, CB, 128], BF16, "xin%d" % b), Reg()) for b in range(2)]
    z1 = [(st.sb([128, CB, 128], BF16, "z1_%d" % b), Reg()) for b in range(2)]
    Yt = ((st.sb([128, CB, 256], BF16, "ytr"), Reg()), (st.sb([128, CB, 256], BF16, "yti"), Reg()))
    Zt = ((st.sb([128, CB, 256], BF16, "zr"), Reg()), (st.sb([128, CB, 256], BF16, "zi"), Reg()))
    Gt = ((st.sb([128, 2, CB, 128], BF16, "gtr"), Reg()), (st.sb([128, 2, CB, 128], BF16, "gti"), Reg()))
    kf = (st.sb([128, 2, CB, 256], BF16, "kf"), Reg())
    mwork = Rot([((st.sb([128, 512], F32, "mA"), Reg()), (st.sb([128, 512], F32, "mB"), Reg())) for _ in range(2)])
    m4 = [(st.sb([128, 512], F32, "m%d" % i), Reg()) for i in range(4)]
    gw = RR([st.sb([128, 512], F32, "gw") for _ in range(2)])
    z2b_all = [(st.sb([128, CB, 128], BF16, "z2b%d" % b), Reg()) for b in range(2)]
    zT = (st.sb([CB, SEQ], BF16, "zT"), Reg())
    pbanks = RR([st.ps() for _ in range(2)])
    xbanks = RR([st.ps() for _ in range(4)])
    tps = RR([st.ps([128, 512], BF16) for _ in range(2)])
    for cb in range(NCB):
        csl = slice(cb * CB, (cb + 1) * CB)
        for b in range(2):
            s.dma("sp", v32[b][0][:], VG[0, b, :, csl, :], writes=[v32[b][1]])
            s.op("pool", lambda e, b=b: e.tensor_copy(out=xin[b][0][:], in_=v32[b][0][:]), reads=[v32[b][1]], writes=[xin[b][1]])
        for o in range(2):
            kft, kfR = kf
            s.dma("act", kft[:].rearrange("a r c b -> a r (c b)"), Kf[o, :, :, csl, :].rearrange("r a c b -> a r (c b)"), writes=[kfR])
            for b in range(2):
                s.dma("sp", g32[b][0][:], VG[1 + o, b, :, csl, :], writes=[g32[b][1]])
            src = xin if o == 0 else z1
            srcs = [(src[0][0], src[0][1], "F1r"), (src[1][0], src[1][1], "F1i")]
            (Zr, ZrR), (Zi, ZiR) = Zt

            def consume(g, xr, xrR, xi, xiR):
                cmul_to(s, xr, xrR, xi, xiR,
                        kft[:, 0, 2 * g:2 * g + 2, :].rearrange("p c b -> p (c b)"), kft[:, 1, 2 * g:2 * g + 2, :].rearrange("p c b -> p (c b)"), kfR, m4,
                        Zr[:, 2 * g:2 * g + 2, :].rearrange("p c b -> p (c b)"), Zi[:, 2 * g:2 * g + 2, :].rearrange("p c b -> p (c b)"), ZrR)
            fft_forward(st, cst, srcs, Yt, pbanks, xbanks, mwork, consume)
            (Gtr, GtrR), (Gti, GtiR) = Gt
            for g in range(CB // 2):
                for j in range(2):
                    p, pR = pbanks.next()

                    def fn(e, p=p, g=g, j=j):
                        ins = None
                        for cc in range(2):
                            c = 2 * g + cc
                            e.matmul(p[:, cc * 256:(cc + 1) * 256], lhsT=Zr[:, c, j * 128:(j + 1) * 128], rhs=I1r[:], start=True, stop=False)
                            ins = e.matmul(p[:, cc * 256:(cc + 1) * 256], lhsT=Zi[:, c, j * 128:(j + 1) * 128], rhs=I1i[:], start=False, stop=True)
                        return ins
                    s.op("pe", fn, reads=[ZrR, I1rR, I1iR], writes=[pR])
                    (mA, mAR), (mB, mBR) = mwork.next()
                    s.op("dve", lambda e, p=p, mA=mA, j=j: e.tensor_tensor(out=mA[:], in0=p[:], in1=ItA[:, j * 512:(j + 1) * 512], op=ALU.mult), reads=[pR, ItAR], writes=[mAR])
                    s.op("dve", lambda e, p=p, mB=mB, j=j: e.tensor_tensor(out=mB[:], in0=p[:], in1=ItB[:, j * 512:(j + 1) * 512], op=ALU.mult), reads=[pR, ItBR], writes=[mBR])
                    mAv = mA[:, :].rearrange("p (c x) -> p c x", c=2)
                    mBv = mB[:, :].rearrange("p (c x) -> p c x", c=2)
                    s.op("pool", lambda e, mAv=mAv, g=g, j=j: e.tensor_tensor(out=Gtr[:, j, 2 * g:2 * g + 2, :], in0=mAv[:, :, 0:128], in1=mAv[:, :, 128:256], op=ALU.subtract), reads=[mAR], writes=[GtrR])
                    s.op("pool", lambda e, mBv=mBv, g=g, j=j: e.tensor_tensor(out=Gti[:, j, 2 * g:2 * g + 2, :], in0=mBv[:, :, 0:128], in1=mBv[:, :, 128:256], op=ALU.add), reads=[mBR], writes=[GtiR])
            for q in range(CB // 4):
                qs = slice(4 * q, 4 * q + 4)
                yr, yrR = xbanks.next()
                yi, yiR = xbanks.next()
                gr = [Gtr[:, j, qs, :].rearrange("p c f -> p (c f)") for j in range(2)]
                gi = [Gti[:, j, qs, :].rearrange("p c f -> p (c f)") for j in range(2)]
                mm_group(s, yr[:], [(I2[:, (j * 3 + 0) * 128:(j * 3 + 1) * 128], gr[j]) for j in range(2)] + [(I2[:, (j * 3 + 2) * 128:(j * 3 + 3) * 128], gi[j]) for j in range(2)],
                         [I2R, GtrR, GtiR], [yrR])
                mm_group(s, yi[:], [(I2[:, (j * 3 + 1) * 128:(j * 3 + 2) * 128], gr[j]) for j in range(2)] + [(I2[:, (j * 3 + 0) * 128:(j * 3 + 1) * 128], gi[j]) for j in range(2)],
                         [I2R, GtrR, GtiR], [yiR])
                for b, (yp, ypR) in enumerate(((yr, yrR), (yi, yiR))):
                    w_, wR_ = gw.next()
                    skip, skipR = (v32[b] if o == 0 else z1[b])
                    dbc = db[:, o, cb * CB + 4 * q:cb * CB + 4 * q + 4].unsqueeze(2).to_broadcast([128, 4, 128])
                    wv = w_[:, :].rearrange("p (c f) -> p c f", c=4)
                    s.op("pool", lambda e, wv=wv, skip=skip, dbc=dbc, qs=qs: e.tensor_tensor(out=wv, in0=skip[:, qs, :], in1=dbc, op=ALU.mult), reads=[skipR, dbR], writes=[wR_])
                    s.op("dve", lambda e, w_=w_, yp=yp: e.tensor_tensor(out=w_[:], in0=yp[:], in1=w_[:], op=ALU.add), reads=[ypR, wR_], writes=[wR_])
                    if o == 0:
                        dst, dstR = z1[b]
                    else:
                        dst, dstR = z2b_all[b]
                    s.op("dve", lambda e, wv=wv, dst=dst, b=b, qs=qs: e.tensor_tensor(out=dst[:, qs, :], in0=wv, in1=g32[b][0][:, qs, :], op=ALU.mult), reads=[wR_, g32[b][1]], writes=[dstR])
            if o == 1:
                for b in range(2):
                    zt, ztR = zT
                    z2t, z2tR = z2b_all[b]
                    for f0 in range(0, 128, 4):
                        tp, tpR = tps.next()

                        def fn(e, tp=tp, f0=f0, z2t=z2t):
                            ins = None
                            for qq in range(4):
                                ins = e.transpose(tp[0:CB, qq * 128:(qq + 1) * 128], z2t[:, :, f0 + qq], ident[:])
                            return ins
                        s.op("pe", fn, reads=[z2tR, identR], writes=[tpR])
                        dstv = zt[:, :].rearrange("c (p f) -> c p f", f=128)[:, :, f0:f0 + 4].rearrange("c p f -> c f p")
                        srcv = tp[0:CB, :].rearrange("c (f p) -> c f p", p=128)
                        if (f0 // 4) % 2:
                            s.op("act", lambda e, dstv=dstv, srcv=srcv: e.activation(out=dstv, in_=srcv, func=AF.Copy), reads=[tpR], writes=[ztR])
                        else:
                            s.op("dve", lambda e, dstv=dstv, srcv=srcv: e.tensor_copy(out=dstv, in_=srcv), reads=[tpR], writes=[ztR])
                    s.dma("sp", z2T[cb * CB:(cb + 1) * CB, b * SEQ:(b + 1) * SEQ], zt[:], reads=[ztR], final=True)
    st.finish()


def _in(nc, name, shape, dt=F32):
    return nc.dram_tensor(name, list(shape), dt, kind="ExternalInput").ap()


def _out(nc, name, shape, dt=F32):
    return nc.dram_tensor(name, list(shape), dt, kind="ExternalOutput").ap()


def _scr(nc, name, shape, dt):
    return nc.dram_tensor(name, list(shape), dt).ap()


def rope_consts():
    inv = (1.0 / (10000.0 ** (np.arange(0, DR, 2, dtype=np.float32) / np.float32(DR)))).astype(np.float32)
    invf = np.concatenate([inv, inv]).reshape(64, 1).astype(np.float32)
    rot = np.zeros((64, 64), np.float32)
    for m in range(32):
        rot[m + 32, m] = -1.0
        rot[m, m + 32] = 1.0
    return invf, rot


def emit_layer0_attn(nc, a, xl1T):
    KT = _scr(nc, "KT", [NH, 128, SEQ], BF16)
    KR = _scr(nc, "KR", [64, SEQ], BF16)
    Vs = _scr(nc, "Vs", [NH, 128, SEQ // 128, 128], BF16)
    QN = _scr(nc, "QN", [NH, 128, TOK], BF16)
    QR = _scr(nc, "QR", [NH, 64, TOK], BF16)
    OT = _scr(nc, "OT", [NH, 128, TOK], BF16)
    stage_kv(nc, "kv", a["xT"], a["pos"], a["mla_w_in"], a["mla_g_kv"], a["mla_w_ukv"], a["invf"], a["rot"], KT, KR, Vs)
    stage_q(nc, "q", a["xTq"], a["posq"], a["mla_w_in"], a["mla_g_q"], a["mla_w_uq"], a["invf"], a["rot"], QN, QR)
    stage_attn(nc, "at", KT, KR, Vs, QN, QR, OT)
    stage_proj_res_ln(nc, "po", OT.rearrange("h p t -> (h p) t"), a["mla_w_o"], a["xTq"], a["ln1_g0"], a["ln1_b0"], xl1T,
                      [(i * 512, 512) for i in range(TOK // 512)])


def build_L1():
    nc = bass.Bass("TRN2", target_bir_lowering=False)
    a = {
        "xT": _in(nc, "xT", [D, SEQ]), "xTq": _in(nc, "xTq", [D, TOK]),
        "pos": _in(nc, "pos", [SEQ], I32), "posq": _in(nc, "posq", [TOK], I32),
        "mla_w_in": _in(nc, "mla_w_in", [D, 704]), "mla_g_q": _in(nc, "mla_g_q", [QL]),
        "mla_w_uq": _in(nc, "mla_w_uq", [QL, 1536]), "mla_g_kv": _in(nc, "mla_g_kv", [KVL]),
        "mla_w_ukv": _in(nc, "mla_w_ukv", [KVL, 2048]), "mla_w_o": _in(nc, "mla_w_o", [D, D]),
        "ln1_g0": _in(nc, "ln1_g0", [D]), "ln1_b0": _in(nc, "ln1_b0", [D]),
        "invf": _in(nc, "invf", [64, 1]), "rot": _in(nc, "rot", [64, 64]),
    }
    xl1T = _out(nc, "xl1T", [D, TOK])
    emit_layer0_attn(nc, a, xl1T)
    return nc


def build_L2(with_bf=True):
    nc = bass.Bass("TRN2", target_bir_lowering=False)
    xl = _in(nc, "xl", [D, TOK + 2])
    w_up = _in(nc, "w_up", [D, 2 * DFF])
    w_conv = _in(nc, "w_conv", [3, 2 * DFF])
    w_down = _in(nc, "w_down", [DFF, D])
    g = _in(nc, "ln_g", [D])
    b = _in(nc, "ln_b", [D])
    outT = _out(nc, "outT", [D, TOK])
    out_bf = _out(nc, "out_bf", [D, TOK], BF16) if with_bf else None
    wup_s = _scr(nc, "wup_s", [NFC, 128, 8, 2, 128], BF16)
    wdn_s = _scr(nc, "wdn_s", [8, 128, NFC, 128], BF16)
    stage_ffn_prep(nc, "fp", w_up, w_down, wup_s, wdn_s)
    stage_ffn(nc, "ff", xl, wup_s, wdn_s, w_conv, g, b, outT, out_bf)
    return nc


def _run(nc, in_maps):
    res = run_bass_kernel_spmd(nc, in_maps, core_ids=list(range(8)))
    return res.results


def run_L1(x, positions, w):
    invf, rot = rope_consts()
    xT = [np.ascontiguousarray(x[b].T) for b in range(2)]
    maps = []
    for c in range(8):
        b, j = c // 4, c % 4
        sl = slice(j * TOK, (j + 1) * TOK)
        m = {"xT": xT[b], "xTq": np.ascontiguousarray(xT[b][:, sl]),
             "pos": np.ascontiguousarray(positions[b]), "posq": np.ascontiguousarray(positions[b][sl]),
             "invf": invf, "rot": rot}
        for k in ("mla_w_in", "mla_g_q", "mla_w_uq", "mla_g_kv", "mla_w_ukv", "mla_w_o"):
            m[k] = np.ascontiguousarray(w[k][0])
        m["ln1_g0"] = np.ascontiguousarray(w["ln1_g"][0])
        m["ln1_b0"] = np.ascontiguousarray(w["ln1_b"][0])
        maps.append(m)
    res = _run(build_L1(), maps)
    return [r["xl1T"] for r in res]


def halo_cols(parts):
    out = []
    for c in range(8):
        b, j = c // 4, c % 4
        z = np.zeros((D, 1), np.float32)
        left = parts[c - 1][:, -1:] if j > 0 else z
        right = parts[c + 1][:, :1] if j < 3 else z
        out.append(np.ascontiguousarray(np.concatenate([left, parts[c], right], axis=1)))
    return out


def run_L2(xl_parts, w, layer, with_bf=True):
    maps = []
    xh = halo_cols(xl_parts)
    for c in range(8):
        maps.append({"xl": xh[c], "w_up": np.ascontiguousarray(w["ffn_w_up"][layer]),
                     "w_conv": np.ascontiguousarray(w["ffn_w_conv"][layer]),
                     "w_down": np.ascontiguousarray(w["ffn_w_down"][layer]),
                     "ln_g": np.ascontiguousarray(w["ln2_g"][layer]), "ln_b": np.ascontiguousarray(w["ln2_b"][layer])})
    res = _run(build_L2(with_bf), maps)
    return [r["outT"] for r in res], ([r["out_bf"] for r in res] if with_bf else None)


HY_W_NAMES = {"fw1": [33, 64], "fw2": [64, 64], "fw3": [64, 64], "fb1": [64, 1], "fb2": [64, 1], "fb3": [64, 1], "freq": [64, 1]}


def emit_hyena(nc, a, z2T):
    UC = _scr(nc, "UC", [3, 2, 128, SEQ], F32)
    VG = _scr(nc, "VG", [3, 2, 128, 128, 128], F32)
    H3 = _scr(nc, "H3", [2, 64, SEQ], F32)
    Kf = _scr(nc, "Kf", [2, 2, 128, 128, 256], BF16)
    stage_hy_proj(nc, "hp", a["x1b"], a["w_in_c"], a["wsh_c"], UC)
    stage_hy_transpose(nc, "ht", UC, a["ident"], VG)
    stage_hy_filter(nc, "hf", a, a, a["fwout_c"], Kf, H3)
    stage_hy_conv(nc, "hc", a, VG, Kf, a["dbias_c"], z2T)


def build_L3():
    nc = bass.Bass("TRN2", target_bir_lowering=False)
    a = {"x1b": _in(nc, "x1b", [D, 2 * SEQ], BF16), "w_in_c": _in(nc, "w_in_c", [D, 3, 128]), "wsh_c": _in(nc, "wsh_c", [128, 9]),
         "fwout_c": _in(nc, "fwout_c", [64, 4, 128]), "dbias_c": _in(nc, "dbias_c", [2, 128])}
    for k, shp in HY_W_NAMES.items():
        a[k] = _in(nc, k, shp)
    for k, shp in HY_CONST_SHAPES.items():
        a[k] = _in(nc, k, shp)
    z2T = _out(nc, "z2T", [128, 2 * SEQ], BF16)
    emit_hyena(nc, a, z2T)
    return nc


def hyena_core_inputs(w, c):
    ch0 = 128 * c
    m = hyena_consts(ch0)
    cols = [j * D + ch0 + np.arange(128) for j in range(3)]
    m["w_in_c"] = np.ascontiguousarray(np.stack([w["hy_w_in"][0][:, cc] for cc in cols], axis=1))
    ws = w["hy_w_short"][0]
    m["wsh_c"] = np.ascontiguousarray(np.stack([ws[k, cols[j]] for j in range(3) for k in range(3)], axis=1))
    fo = w["hy_fw_out"][0]
    m["fwout_c"] = np.ascontiguousarray(np.stack([fo[:, od * D + ch0:od * D + ch0 + 128] for od in range(4)], axis=1))
    m["dbias_c"] = np.ascontiguousarray(w["hy_d_bias"][0][:, ch0:ch0 + 128])
    m["fw1"] = np.ascontiguousarray(w["hy_fw1"][0])
    m["fw2"] = np.ascontiguousarray(w["hy_fw2"][0])
    m["fw3"] = np.ascontiguousarray(w["hy_fw3"][0])
    for k in ("fb1", "fb2", "fb3", "freq"):
        m[k] = np.ascontiguousarray(w["hy_" + k][0].reshape(64, 1))
    return m


def run_L3(x1bf_parts, w):
    x1b = np.ascontiguousarray(np.concatenate(x1bf_parts, axis=1))
    maps = []
    for c in range(8):
        m = hyena_core_inputs(w, c)
        m["x1b"] = x1b
        maps.append(m)
    res = _run(build_L3(), maps)
    return [r["z2T"] for r in res]


def build_L4():
    nc = bass.Bass("TRN2", target_bir_lowering=False)
    z2h = _in(nc, "z2h", [D, TOK + 2], BF16)
    x1h = _in(nc, "x1h", [D, TOK + 2])
    mask = _in(nc, "mask", [128, 2])
    w_o = _in(nc, "w_o", [D, D])
    g1 = _in(nc, "ln1_g", [D])
    b1 = _in(nc, "ln1_b", [D])
    w_up = _in(nc, "w_up", [D, 2 * DFF])
    w_conv = _in(nc, "w_conv", [3, 2 * DFF])
    w_down = _in(nc, "w_down", [DFF, D])
    g = _in(nc, "ln_g", [D])
    b = _in(nc, "ln_b", [D])
    outT = _out(nc, "outT", [D, TOK])
    xl = _scr(nc, "xl", [D, TOK + 2], F32)
    wup_s = _scr(nc, "wup_s", [NFC, 128, 8, 2, 128], BF16)
    wdn_s = _scr(nc, "wdn_s", [8, 128, NFC, 128], BF16)
    tiles = [(TOK, 0, 1)] + [(i * 512, 1 + i * 512, 512) for i in range(TOK // 512)] + [(TOK + 1, TOK + 1, 1)]
    stage_proj_res_ln(nc, "cp", z2h, w_o, x1h, g1, b1, xl, tiles, mask=mask, mask_cols={0: 0, len(tiles) - 1: 1})
    stage_ffn_prep(nc, "fp", w_up, w_down, wup_s, wdn_s)
    stage_ffn(nc, "ff", xl, wup_s, wdn_s, w_conv, g, b, outT, None)
    return nc


def halo_cols_generic(full_by_batch, dtype):
    out = []
    for c in range(8):
        b, j = c // 4, c % 4
        buf = np.zeros((D, TOK + 2), dtype)
        buf[:, :TOK] = full_by_batch[b][:, j * TOK:(j + 1) * TOK]
        if j > 0:
            buf[:, TOK] = full_by_batch[b][:, j * TOK - 1]
        if j < 3:
            buf[:, TOK + 1] = full_by_batch[b][:, (j + 1) * TOK]
        out.append(buf)
    return out


def run_L4(z2_parts, x1_parts, w):
    z2full = np.concatenate(z2_parts, axis=0)
    z2b = [z2full[:, b * SEQ:(b + 1) * SEQ] for b in range(2)]
    x1b = [np.concatenate(x1_parts[b * 4:(b + 1) * 4], axis=1) for b in range(2)]
    z2h = halo_cols_generic(z2b, z2full.dtype)
    x1h = halo_cols_generic(x1b, np.float32)
    maps = []
    for c in range(8):
        j = c % 4
        mask = np.ones((128, 2), np.float32)
        if j == 0:
            mask[:, 0] = 0.0
        if j == 3:
            mask[:, 1] = 0.0
        maps.append({"z2h": z2h[c], "x1h": x1h[c], "mask": mask, "w_o": np.ascontiguousarray(w["hy_w_o"][0]),
                     "ln1_g": np.ascontiguousarray(w["ln1_g"][1]), "ln1_b": np.ascontiguousarray(w["ln1_b"][1]),
                     "w_up": np.ascontiguousarray(w["ffn_w_up"][1]), "w_conv": np.ascontiguousarray(w["ffn_w_conv"][1]),
                     "w_down": np.ascontiguousarray(w["ffn_w_down"][1]),
                     "ln_g": np.ascontiguousarray(w["ln2_g"][1]), "ln_b": np.ascontiguousarray(w["ln2_b"][1])})
    res = _run(build_L4(), maps)
    return [r["outT"] for r in res]


def kernel(**inputs):
    w = {k: np.asarray(v) for k, v in inputs.items()}
    x = np.ascontiguousarray(w["x"], dtype=np.float32)
    positions = np.ascontiguousarray(w["positions"], dtype=np.int32)
    xl1 = run_L1(x, positions, w)
    x1, x1bf = run_L2(xl1, w, 0, True)
    z2 = run_L3(x1bf, w)
    outs = run_L4(z2, x1, w)
    out = np.empty((2, SEQ, D), np.float32)
    for c in range(8):
        b, j = c // 4, c % 4
        out[b, j * TOK:(j + 1) * TOK, :] = outs[c].T
    return out
```
